# Optimizing a Trainium2 kernel written in Bass

```python
import jax
import jax.numpy as jnp
from jax import lax
import numpy as np

D_MODEL = 1024
BATCH = 8
SEQ = 4096
DEPTH = 4

CHUNK = 64
N_MIXERS = 2
N_A_LAYERS = (DEPTH + 1) // 2
N_B_LAYERS = DEPTH // 2

GDN_QK_HEADS = 8
GDN_V_HEADS = 16
GDN_HEAD_DIM = 128
GDN_KEY_DIM = GDN_QK_HEADS * GDN_HEAD_DIM
GDN_VAL_DIM = GDN_V_HEADS * GDN_HEAD_DIM
GDN_CONV = 4
GDN_CONV_DIM = 2 * GDN_KEY_DIM + GDN_VAL_DIM
GDN_IN_DIM = GDN_CONV_DIM + GDN_VAL_DIM + 2 * GDN_V_HEADS

SB_HEADS = 16
SB_HEAD_DIM = 64
SB_DIM = SB_HEADS * SB_HEAD_DIM
SB_Q_BLOCK = 128

N_EXPERTS = 32
TOP_K = 4
D_FF = D_MODEL
SWIGLU_LIMIT = 7.0
SWIGLU_ALPHA = 1.702
MOE_BLOCK = 256

DN_ALPHA = (2 * DEPTH) ** 0.25
DN_BETA = (8 * DEPTH) ** -0.25
LN_EPS = 1e-5
RMS_EPS = 1e-6
L2_EPS = 1e-6

kernel_name = "hybrid_gdn_stickbreak_moe_deepnorm"


def layer_norm(x, g, b):
    xf = x.astype(jnp.float32)
    mu = jnp.mean(xf, axis=-1, keepdims=True)
    var = jnp.mean(jnp.square(xf - mu), axis=-1, keepdims=True)
    return ((xf - mu) * lax.rsqrt(var + LN_EPS) * g.astype(jnp.float32) + b.astype(jnp.float32)).astype(x.dtype)


def l2_normalize(t):
    tf = t.astype(jnp.float32)
    return tf * lax.rsqrt(jnp.sum(tf * tf, axis=-1, keepdims=True) + L2_EPS)


def causal_depthwise_conv(x, w):
    k, c = w.shape
    return lax.conv_general_dilated(
        x, w[:, None, :].astype(x.dtype), window_strides=(1,), padding=[(k - 1, 0)],
        dimension_numbers=('NWC', 'WIO', 'NWC'), feature_group_count=c)


def chunk_gated_delta_rule(q, k, v, g, beta):
    f32 = jnp.float32
    b, h, s, dk = q.shape
    dv = v.shape[-1]
    nc = s // CHUNK

    def chunks(t):
        return t.astype(f32).reshape(b, h, nc, CHUNK, *t.shape[3:])

    q, k, v, g, beta = chunks(q), chunks(k), chunks(v), chunks(g), chunks(beta)
    g = jnp.cumsum(g, axis=-1)
    pos = jnp.arange(CHUNK)
    incl = pos[:, None] >= pos[None, :]
    strict = pos[:, None] > pos[None, :]
    decay = jnp.exp(jnp.where(incl, g[..., :, None] - g[..., None, :], -jnp.inf))
    k_beta = k * beta[..., None]
    a_strict = jnp.where(strict, jnp.einsum('bhncd,bhnsd->bhncs', k_beta, k) * decay, 0.0)
    eye = jnp.broadcast_to(jnp.eye(CHUNK, dtype=f32), a_strict.shape)
    t_inv = lax.linalg.triangular_solve(a_strict, eye, left_side=True, lower=True, unit_diagonal=True)
    u = jnp.einsum('bhncs,bhnsd->bhncd', t_inv, v * beta[..., None])
    w = jnp.einsum('bhncs,bhnsd->bhncd', t_inv, k_beta * jnp.exp(g)[..., None])
    q_dec = q * jnp.exp(g)[..., None]
    attn = jnp.einsum('bhncd,bhnsd->bhncs', q, k) * decay
    g_last = g[..., -1:]
    k_dec = k * jnp.exp(g_last - g)[..., None]
    chunk_decay = jnp.exp(g_last[..., 0])

    def step(state, xs):
        q_c, k_c, u_c, w_c, a_c, d_c = xs
        v_new = u_c - jnp.einsum('bhcd,bhde->bhce', w_c, state)
        o_c = jnp.einsum('bhcd,bhde->bhce', q_c, state) + jnp.einsum('bhcs,bhse->bhce', a_c, v_new)
        state = state * d_c[..., None, None] + jnp.einsum('bhcd,bhce->bhde', k_c, v_new)
        return state, o_c

    xs = (jnp.moveaxis(q_dec, 2, 0), jnp.moveaxis(k_dec, 2, 0), jnp.moveaxis(u, 2, 0),
          jnp.moveaxis(w, 2, 0), jnp.moveaxis(attn, 2, 0), jnp.moveaxis(chunk_decay, 2, 0))
    state0 = jnp.zeros((b, h, dk, dv), f32)
    _, o = lax.scan(step, state0, xs)
    return jnp.moveaxis(o, 0, 2).reshape(b, h, s, dv)


def gated_deltanet(x, w_in, conv_w, a_log, dt_bias, norm_g, w_out):
    f32 = jnp.float32
    b, s, _ = x.shape
    proj = x @ w_in
    o1 = GDN_CONV_DIM
    o2 = o1 + GDN_VAL_DIM
    o3 = o2 + GDN_V_HEADS
    qkv, z, b_raw, a_raw = proj[..., :o1], proj[..., o1:o2], proj[..., o2:o3], proj[..., o3:]
    qkv = jax.nn.silu(causal_depthwise_conv(qkv, conv_w))
    q = qkv[..., :GDN_KEY_DIM].reshape(b, s, GDN_QK_HEADS, GDN_HEAD_DIM)
    k = qkv[..., GDN_KEY_DIM:2 * GDN_KEY_DIM].reshape(b, s, GDN_QK_HEADS, GDN_HEAD_DIM)
    v = qkv[..., 2 * GDN_KEY_DIM:].reshape(b, s, GDN_V_HEADS, GDN_HEAD_DIM)
    q = l2_normalize(q) * (GDN_HEAD_DIM ** -0.5)
    k = l2_normalize(k)
    rep = GDN_V_HEADS // GDN_QK_HEADS
    q = jnp.repeat(q, rep, axis=2)
    k = jnp.repeat(k, rep, axis=2)
    beta = jax.nn.sigmoid(b_raw.astype(f32))
    g = -jnp.exp(a_log.astype(f32)) * jax.nn.softplus(a_raw.astype(f32) + dt_bias.astype(f32))
    o = chunk_gated_delta_rule(jnp.swapaxes(q, 1, 2), jnp.swapaxes(k, 1, 2), jnp.swapaxes(v, 1, 2),
                               jnp.swapaxes(g, 1, 2), jnp.swapaxes(beta, 1, 2))
    o = jnp.swapaxes(o, 1, 2)
    zf = z.astype(f32).reshape(b, s, GDN_V_HEADS, GDN_HEAD_DIM)
    o = o * lax.rsqrt(jnp.mean(o * o, axis=-1, keepdims=True) + RMS_EPS) * norm_g.astype(f32) * jax.nn.silu(zf)
    return o.reshape(b, s, GDN_VAL_DIM).astype(x.dtype) @ w_out


def stick_breaking_attention(x, w_qkv, w_out):
    f32 = jnp.float32
    b, s, _ = x.shape
    qkv = x @ w_qkv

    def heads(t):
        return t.reshape(b, s, SB_HEADS, SB_HEAD_DIM).transpose(0, 2, 1, 3)

    q = heads(qkv[..., :SB_DIM])
    k = heads(qkv[..., SB_DIM:2 * SB_DIM])
    v = heads(qkv[..., 2 * SB_DIM:])
    scale = SB_HEAD_DIM ** -0.5
    outs = []
    for blk in range(s // SB_Q_BLOCK):
        t0 = blk * SB_Q_BLOCK
        t1 = t0 + SB_Q_BLOCK
        z = jnp.einsum('bhtd,bhsd->bhts', q[:, :, t0:t1], k[:, :, :t1]).astype(f32) * scale
        causal = jnp.arange(t1)[None, :] < (t0 + jnp.arange(SB_Q_BLOCK))[:, None]
        log_keep = jnp.where(causal, jax.nn.log_sigmoid(-z), 0.0)
        after = lax.cumsum(log_keep, axis=3, reverse=True) - log_keep
        wts = jnp.exp(jnp.where(causal, jax.nn.log_sigmoid(z) + after, -jnp.inf))
        outs.append(jnp.einsum('bhts,bhsd->bhtd', wts.astype(v.dtype), v[:, :, :t1]))
    o = jnp.concatenate(outs, axis=2)
    return o.transpose(0, 2, 1, 3).reshape(b, s, SB_DIM) @ w_out


def moe_ffn(x, w_router, b_router, w_gu, b_gu, w_down, b_down):
    b, s, d = x.shape
    n_tok = b * s
    n_pairs = n_tok * TOP_K
    xt = x.reshape(n_tok, d)
    logits = (xt @ w_router + b_router).astype(jnp.float32)
    top_logits, top_idx = lax.top_k(logits, TOP_K)
    gates = jax.nn.softmax(top_logits, axis=-1)
    flat_e = top_idx.reshape(-1)
    flat_tok = jnp.repeat(jnp.arange(n_tok, dtype=jnp.int32), TOP_K)
    flat_gate = gates.reshape(-1)
    order = jnp.argsort(flat_e)
    e_sorted, tok_sorted, gate_sorted = flat_e[order], flat_tok[order], flat_gate[order]
    counts = jnp.bincount(flat_e, length=N_EXPERTS)
    padded = (counts + MOE_BLOCK - 1) // MOE_BLOCK * MOE_BLOCK
    starts = jnp.cumsum(counts) - counts
    pad_ends = jnp.cumsum(padded)
    pad_starts = pad_ends - padded
    dest = pad_starts[e_sorted] + (jnp.arange(n_pairs) - starts[e_sorted])
    n_blocks = -(-n_pairs // MOE_BLOCK) + N_EXPERTS
    n_rows = n_blocks * MOE_BLOCK
    rows_tok = jnp.zeros((n_rows,), jnp.int32).at[dest].set(tok_sorted)
    rows_gate = jnp.zeros((n_rows,), x.dtype).at[dest].set(gate_sorted.astype(x.dtype))
    x_rows = xt[rows_tok].reshape(n_blocks, MOE_BLOCK, d)
    block_expert = jnp.minimum(
        jnp.searchsorted(pad_ends, jnp.arange(n_blocks) * MOE_BLOCK, side='right'), N_EXPERTS - 1)

    def expert_block(args):
        xb, e = args
        hgu = xb @ w_gu[e] + b_gu[e]
        gate = jnp.minimum(hgu[:, 0::2], SWIGLU_LIMIT)
        up = jnp.clip(hgu[:, 1::2], -SWIGLU_LIMIT, SWIGLU_LIMIT)
        act = (up + 1.0) * gate * jax.nn.sigmoid(SWIGLU_ALPHA * gate)
        return act @ w_down[e] + b_down[e]

    y_rows = lax.map(expert_block, (x_rows, block_expert)).reshape(n_rows, d)
    out = jnp.zeros((n_tok, d), x.dtype).at[rows_tok].add(y_rows * rows_gate[:, None])
    return out.reshape(b, s, d)


def setup_inputs(seed: int = 0) -> dict:
    key = jax.random.key(seed)
    ks = jax.random.split(key, 20)
    f32 = jnp.float32

    def nrm(k, shape, scale):
        return jax.random.normal(k, shape, f32) * scale

    na, nb, nl = N_A_LAYERS, N_B_LAYERS, DEPTH
    dt = jnp.exp(jax.random.uniform(ks[4], (na, GDN_V_HEADS), f32, np.log(1e-3), np.log(1e-1)))
    dt_bias = dt + jnp.log(-jnp.expm1(-dt))
    return {
        "x": nrm(ks[0], (BATCH, SEQ, D_MODEL), 1.0),
        "gdn_w_in": nrm(ks[1], (na, D_MODEL, GDN_IN_DIM), D_MODEL ** -0.5),
        "gdn_conv": nrm(ks[2], (na, GDN_CONV, GDN_CONV_DIM), GDN_CONV ** -0.5),
        "gdn_a_log": jnp.log(jax.random.uniform(ks[3], (na, GDN_V_HEADS), f32, 1.0, 16.0)),
        "gdn_dt_bias": dt_bias,
        "gdn_norm_g": 1.0 + nrm(ks[5], (na, GDN_HEAD_DIM), 0.02),
        "gdn_w_out": nrm(ks[6], (na, GDN_VAL_DIM, D_MODEL), GDN_VAL_DIM ** -0.5 * DN_BETA),
        "sb_w_qkv": nrm(ks[7], (nb, D_MODEL, 3 * SB_DIM), D_MODEL ** -0.5),
        "sb_w_out": nrm(ks[8], (nb, SB_DIM, D_MODEL), SB_DIM ** -0.5 * DN_BETA),
        "ln1_g": 1.0 + nrm(ks[9], (nl, D_MODEL), 0.02),
        "ln1_b": nrm(ks[10], (nl, D_MODEL), 0.02),
        "moe_w_router": nrm(ks[11], (nl, D_MODEL, N_EXPERTS), D_MODEL ** -0.5),
        "moe_b_router": nrm(ks[12], (nl, N_EXPERTS), 0.01),
        "moe_w_gu": nrm(ks[13], (nl, N_EXPERTS, D_MODEL, 2 * D_FF), D_MODEL ** -0.5),
        "moe_b_gu": nrm(ks[14], (nl, N_EXPERTS, 2 * D_FF), 0.02),
        "moe_w_down": nrm(ks[15], (nl, N_EXPERTS, D_FF, D_MODEL), D_FF ** -0.5 * DN_BETA),
        "moe_b_down": nrm(ks[16], (nl, N_EXPERTS, D_MODEL), 0.02),
        "ln2_g": 1.0 + nrm(ks[17], (nl, D_MODEL), 0.02),
        "ln2_b": nrm(ks[18], (nl, D_MODEL), 0.02),
    }


def reference(x, gdn_w_in, gdn_conv, gdn_a_log, gdn_dt_bias, gdn_norm_g, gdn_w_out,
              sb_w_qkv, sb_w_out, ln1_g, ln1_b, moe_w_router, moe_b_router,
              moe_w_gu, moe_b_gu, moe_w_down, moe_b_down, ln2_g, ln2_b):
    for i in range(DEPTH):
        j = i // N_MIXERS
        if i % N_MIXERS == 0:
            h = gated_deltanet(x, gdn_w_in[j], gdn_conv[j], gdn_a_log[j], gdn_dt_bias[j],
                               gdn_norm_g[j], gdn_w_out[j])
        else:
            h = stick_breaking_attention(x, sb_w_qkv[j], sb_w_out[j])
        x = layer_norm(DN_ALPHA * x + h, ln1_g[i], ln1_b[i])
        f = moe_ffn(x, moe_w_router[i], moe_b_router[i], moe_w_gu[i], moe_b_gu[i],
                    moe_w_down[i], moe_b_down[i])
        x = layer_norm(DN_ALPHA * x + f, ln2_g[i], ln2_b[i])
    return x
```

```python
import numpy as np
from contextlib import ExitStack
import concourse.bass as bass
import concourse.mybir as mybir
from concourse.bass_utils import run_bass_kernel_spmd

F32 = mybir.dt.float32
BF16 = mybir.dt.bfloat16
I32 = mybir.dt.int32
ALU = mybir.AluOpType
AF = mybir.ActivationFunctionType
AX = mybir.AxisListType

ENGS = ("pe", "dve", "act", "pool", "sp")
N_DMA_SLOTS = 48

T = 4096
D = 1024
NT = T // 128
NE = 32
BLK = 256
NBLK = T * 4 // BLK + NE
NROWS = NBLK * BLK
ALPHA = 8.0 ** 0.25
LN_EPS = 1e-5


class Prog:
    def __init__(self, nc):
        self.nc = nc
        self.ops = []
        self.lastw = {}
        self.readers = {}
        self.dma_n = 0
        self.dma_n_pool = 0
        self.slot_last = [None] * N_DMA_SLOTS
        self.slot_cnt = [0] * N_DMA_SLOTS
        self.bar = None
        self.bar_done = set()
        self.last_on = {}

    def barrier(self):
        deps = [i for i in self.last_on.values()]
        deps += [i for i in self.slot_last if i is not None]
        self.bar = deps
        self.bar_done = set()

    def op(self, eng, fn, reads=(), writes=(), dma=False):
        psb = [("ps", k[1]) for k in list(reads) + list(writes) if isinstance(k, tuple) and k[0] == "ps"]
        if psb:
            reads = [k for k in reads if not (isinstance(k, tuple) and k[0] == "ps")]
            writes = [k for k in writes if not (isinstance(k, tuple) and k[0] == "ps")] + sorted(set(psb))
        deps = {}
        for r in reads:
            t = self.lastw.get(r)
            if t is not None:
                deps[t] = True
        for w in writes:
            t = self.lastw.get(w)
            if t is not None:
                deps.setdefault(t, False)
            for t in self.readers.get(w, ()):
                deps.setdefault(t, False)
        if self.bar is not None and eng not in self.bar_done:
            for t in self.bar:
                deps[t] = True
            self.bar_done.add(eng)
        idx = len(self.ops)
        rec = dict(eng=eng, fn=fn, deps=deps, dma=dma, sig=False, slot=None, slotval=0)
        if dma:
            half = N_DMA_SLOTS // 2
            if eng == "pool":
                s = half + self.dma_n_pool % half
                self.dma_n_pool += 1
            else:
                s = self.dma_n % half
                self.dma_n += 1
            prev = self.slot_last[s]
            if prev is not None:
                deps[prev] = True
            self.slot_cnt[s] += 1
            rec["slot"] = s
            rec["slotval"] = 16 * self.slot_cnt[s]
            self.slot_last[s] = idx
        else:
            self.last_on[eng] = idx
        self.ops.append(rec)
        for w in writes:
            self.lastw[w] = idx
            self.readers[w] = []
        for r in reads:
            if r not in writes:
                self.readers.setdefault(r, []).append(idx)
        return idx

    def emit(self, es):
        nc = self.nc
        ops = self.ops
        for o in ops:
            nd = set()
            for d, raw in o["deps"].items():
                do = ops[d]
                nd.add(d)
                if not do["dma"]:
                    do["sig"] = True
            o["deps"] = nd
        csem = {e: es.enter_context(nc.semaphore("c_" + e)) for e in ENGS}
        dsem = [es.enter_context(nc.semaphore("d%d" % i)) for i in range(N_DMA_SLOTS)]
        cnt = {e: 0 for e in ENGS}
        for o in ops:
            if not o["dma"] and o["sig"]:
                cnt[o["eng"]] += 1
                o["sigval"] = cnt[o["eng"]]
        engobj = {"pe": nc.tensor, "dve": nc.vector, "act": nc.scalar, "pool": nc.gpsimd, "sp": nc.sync}
        per = {e: [o for o in ops if o["eng"] == e] for e in ENGS}
        final_dma = [(s, 16 * self.slot_cnt[s]) for s in range(N_DMA_SLOTS) if self.slot_cnt[s]]
        final_c = dict(cnt)

        def body_for(e):
            def body(eng_):
                eng = engobj[e]
                seen = {}
                for o in per[e]:
                    for d in sorted(o["deps"]):
                        do = ops[d]
                        if do["dma"]:
                            key = ("d", do["slot"]); val = do["slotval"]; sem = dsem[do["slot"]]
                        else:
                            key = ("c", do["eng"]); val = do["sigval"]; sem = csem[do["eng"]]
                        if seen.get(key, 0) < val:
                            eng.wait_ge(sem, val)
                            seen[key] = val
                    inst = o["fn"](eng)
                    if o["dma"]:
                        inst.then_inc(dsem[o["slot"]], 16)
                    elif o["sig"]:
                        inst.then_inc(csem[e], 1)
                if e == "sp":
                    for s, v in final_dma:
                        eng.wait_ge(dsem[s], v)
                    for e2, v in final_c.items():
                        if v:
                            eng.wait_ge(csem[e2], v)
            return body

        with nc.Block() as block:
            block.tensor(body_for("pe"))
            block.vector(body_for("dve"))
            block.scalar(body_for("act"))
            block.gpsimd(body_for("pool"))
            block.sync(body_for("sp"))
        return {e: len(per[e]) for e in ENGS}, cnt


class Arena:
    def __init__(self, ap_f32, words):
        self.ap = ap_f32
        self.words = words
        self.off = 0

    def reset(self, off=0):
        self.off = off

    def alloc(self, shape, dtype=F32, parts=128):
        n = int(np.prod(shape))
        if dtype == BF16:
            w = (n + 1) // 2
        else:
            w = n
        w = (w + 7) // 8 * 8
        assert self.off + w <= self.words, ("arena overflow", self.off, w, self.words)
        a = self.ap[0:parts, self.off:self.off + w]
        self.off += w
        if dtype != F32:
            a = a.bitcast(dtype)
        a = a[:, 0:n]
        if len(shape) == 2:
            a = a.rearrange("p (a b) -> p a b", a=shape[0], b=shape[1])
        elif len(shape) == 3:
            a = a.rearrange("p (a b c) -> p a b c", a=shape[0], b=shape[1], c=shape[2])
        return a


class Builder:
    def __init__(self, nc, es, arena_kib=150):
        self.nc = nc
        self.es = es
        self.P = Prog(nc)
        self.ps = es.enter_context(nc.psum_tensor("ps", [128, 8, 512], F32))
        self.bank = 0
        words = arena_kib * 256
        self.arena_t = es.enter_context(nc.sbuf_tensor("arena", [128, words], F32))
        self.A = Arena(self.arena_t, words)
        self.uid = 0
        self.consts()

    def sb(self, name, shape, dtype=F32):
        return self.es.enter_context(self.nc.sbuf_tensor(name, shape, dtype))

    def nb(self):
        b = self.bank
        self.bank = (self.bank + 1) % 8
        return b

    def key(self, s):
        self.uid += 1
        return (s, self.uid)

    def dma(self, out, in_, reads, writes, eng="sp", **kw):
        self.P.op(eng, lambda e: e.dma_start(out=out, in_=in_, **kw), reads=reads, writes=writes, dma=True)

    def mm(self, out, pairs, reads, writes):
        def fn(e):
            n = len(pairs)
            inst = None
            for i, (l, r) in enumerate(pairs):
                inst = e.matmul(out, lhsT=l, rhs=r, start=(i == 0), stop=(i == n - 1))
            return inst
        self.P.op("pe", fn, reads=reads, writes=writes)

    def tr(self, out, in_, ident, reads, writes):
        self.P.op("pe", lambda e: e.transpose(out, in_, ident), reads=reads, writes=writes)

    def v(self, eng, name, reads, writes, **kw):
        self.P.op(eng, lambda e: getattr(e, name)(**kw), reads=reads, writes=writes)

    def consts(self):
        nc, P = self.nc, self.P
        self.ident_f = self.sb("ident_f", [128, 128], F32)
        self.ident_b = self.sb("ident_b", [128, 128], BF16)
        self.ones_f = self.sb("ones_f", [128, 128], F32)
        self.ustrict = self.sb("ustrict", [128, 128], F32)
        self.iota_p = self.sb("iota_p", [128, 256], F32)
        self.iota_i = self.sb("iota_i", [128, 256], I32)
        self.v("pool", "memset", [], ["ones_f"], ap=self.ones_f[:], constant=1.0)
        self.v("pool", "affine_select", ["ones_f"], ["ident_f"], out=self.ident_f[:], in_=self.ones_f[:],
               pattern=[[-1, 128]], compare_op=ALU.is_equal, fill=0.0, base=0, channel_multiplier=1)
        self.v("dve", "tensor_copy", ["ident_f"], ["ident_b"], out=self.ident_b[:], in_=self.ident_f[:])
        self.v("pool", "affine_select", ["ones_f"], ["ustrict"], out=self.ustrict[:], in_=self.ones_f[:],
               pattern=[[1, 128]], compare_op=ALU.is_gt, fill=0.0, base=0, channel_multiplier=-1)
        self.v("pool", "iota", [], ["iota_i"], out=self.iota_i[:], pattern=[[0, 256]], base=0, channel_multiplier=1)
        self.v("dve", "tensor_copy", ["iota_i"], ["iota_p"], out=self.iota_p[:], in_=self.iota_i[:])


class Buf:
    def __init__(self, ap, key):
        self.ap = ap
        self.key = key


def psk(b, q=None, h=None):
    if q is not None:
        return [("ps", b, q)]
    if h is not None:
        return [("ps", b, 2 * h), ("ps", b, 2 * h + 1)]
    return [("ps", b, i) for i in range(4)]


class MoEMixin:
    def abuf(self, name, shape, dtype=F32):
        return Buf(self.A.alloc(shape, dtype), name)

    def alloc_moe_state(self):
        self.logits = self.sb("logits", [128, NT, NE], F32)
        self.max8 = self.sb("max8", [128, NT, 8], F32)
        self.pos = self.sb("pos", [128, NT, NE], F32)
        self.run_bc = self.sb("run_bc", [128, NE], F32)
        self.gates = self.sb("gates", [128, NT, 4], F32)
        self.dest_i = self.sb("dest_i", [128, NT, 4], I32)
        self.widx_i = self.sb("widx_i", [128, NBLK, 8], I32)
        self.be = self.sb("be", [128, NBLK], F32)
        self.pad_starts = self.sb("pad_starts", [128, NE], F32)
        self.pad_ends = self.sb("pad_ends", [128, NE], F32)
        self.wr = self.sb("wr", [128, 8, NE], F32)
        self.br = self.sb("br", [1, NE], F32)
        self.bgu = self.sb("bgu", [NE, 2048], F32)
        self.bdn = self.sb("bdn", [NE, D], F32)
        self.lng = self.sb("lng", [128, 1, D], F32)
        self.lnb = self.sb("lnb", [128, 1, D], F32)
        self.thr = self.sb("thr", [128, NBLK], F32)
        self.thr_i = self.sb("thr_i", [128, NBLK], I32)
        self.cbase = self.sb("cbase", [128, 8], F32)
        self.cbase_i = self.sb("cbase_i", [128, 8], I32)
        self.ones32 = self.sb("ones32", [128, NE], F32)
        self.v("pool", "iota", [], ["thr_i"], out=self.thr_i[:], pattern=[[BLK, NBLK]], base=0, channel_multiplier=0)
        self.v("dve", "tensor_copy", ["thr_i"], ["thr"], out=self.thr[:], in_=self.thr_i[:])
        self.v("pool", "iota", [], ["cbase_i"], out=self.cbase_i[:], pattern=[[128, 8]], base=0, channel_multiplier=1)
        self.v("dve", "tensor_copy", ["cbase_i"], ["cbase"], out=self.cbase[:], in_=self.cbase_i[:])
        self.v("pool", "memset", [], ["ones32"], ap=self.ones32[:], constant=1.0)

    def zero_rows(self, xrows):
        zb = self.abuf("zrows", [2, D], BF16)
        z = zb.ap
        self.v("pool", "memset", [], ["zrows"], ap=z, constant=0.0)
        for b in range(NBLK):
            self.dma(xrows[b * BLK:(b + 1) * BLK, :].rearrange("(i p) d -> p i d", p=128), z, ["zrows"],
                     [("xrows_sc", t, j) for t in range(NT) for j in range(4)] if b == NBLK - 1 else [("xrows_z", b)])

    def load_layer_small(self, l, W):
        self.dma(self.wr[:], W["moe_w_router"][l].rearrange("(kc p) e -> p kc e", p=128), [], ["wr"])
        self.dma(self.br[:], W["moe_b_router"][l:l + 1, :], [], ["br"])
        self.dma(self.bgu[:], W["moe_b_gu"][l], [], ["bgu"])
        self.dma(self.bdn[:], W["moe_b_down"][l], [], ["bdn"])
        self.v("pool", "memset", [], ["run_bc"], ap=self.run_bc[:], constant=0.0)
        self.Wd = W

    def load_ln(self, l, which):
        g, b = (("ln1_g", "ln1_b"), ("ln2_g", "ln2_b"))[which]
        self.dma(self.lng[:, 0:1, :], self.Wd[g][l:l + 1, :].partition_broadcast(128), [], ["lng"])
        self.dma(self.lnb[:, 0:1, :], self.Wd[b][l:l + 1, :].partition_broadcast(128), [], ["lnb"])

    def ln_tmp(self, tag):
        return dict(st=self.abuf("st" + tag, [2, 6]), mv=self.abuf("mv" + tag, [2]), rstd=self.abuf("rstd" + tag, [1]))

    def ln_tile(self, y, which, out, tmp):
        st, mv, rstd = tmp["st"], tmp["mv"], tmp["rstd"]
        self.v("dve", "bn_stats", [y.key], [(st.key, 0)], out=st.ap[:, 0, :], in_=y.ap[:, 0:512])
        self.v("dve", "bn_stats", [y.key], [(st.key, 1)], out=st.ap[:, 1, :], in_=y.ap[:, 512:1024])
        self.v("dve", "bn_aggr", [(st.key, 0), (st.key, 1)], [mv.key], out=mv.ap, in_=st.ap)
        self.v("act", "activation", [mv.key], [rstd.key], out=rstd.ap, in_=mv.ap[:, 1:2], func=AF.Sqrt, bias=LN_EPS, scale=1.0)
        self.v("dve", "reciprocal", [rstd.key], [rstd.key], out=rstd.ap, in_=rstd.ap)
        self.v("dve", "tensor_scalar", [y.key, mv.key, rstd.key], [out.key], out=out.ap, in0=y.ap, scalar1=mv.ap[:, 0:1], scalar2=rstd.ap[:, 0:1],
               op0=ALU.subtract, op1=ALU.mult)
        self.v("pool", "tensor_tensor", [out.key, "lng"], [out.key], out=out.ap, in0=out.ap, in1=self.lng[:, 0, :], op=ALU.mult)
        self.v("pool", "tensor_tensor", [out.key, "lnb"], [out.key], out=out.ap, in0=out.ap, in1=self.lnb[:, 0, :], op=ALU.add)

    def route_tile(self, t, xn, xT, mask):
        ps = self.ps
        for half in range(2):
            b = self.nb()
            for q in range(4):
                kc = half * 4 + q
                self.tr(ps[:, b, q * 128:(q + 1) * 128], xn.ap[:, kc * 128:(kc + 1) * 128], self.ident_f[:],
                        [xn.key, "ident_f"], psk(b, q))
            self.v("act", "activation", psk(b), [(xT.key, half)],
                   out=xT.ap[:, half * 4:(half + 1) * 4, :], in_=ps[:, b, :].rearrange("p (a c) -> p a c", a=4), func=AF.Copy)
        b = self.nb()
        pairs = [(xT.ap[:, kc, :], self.wr[:, kc, :]) for kc in range(8)]
        pairs.append((self.ones_f[0:1, 0:128], self.br[0:1, :]))
        self.mm(ps[:, b, 0:NE], pairs, [(xT.key, 0), (xT.key, 1), "wr", "br", "ones_f"], psk(b))
        lk = ("logits", t)
        self.v("act", "activation", psk(b), [lk], out=self.logits[:, t, :], in_=ps[:, b, 0:NE], func=AF.Copy)
        mk8 = ("max8", t)
        self.v("dve", "max", [lk], [mk8], out=self.max8[:, t, :], in_=self.logits[:, t, :])
        self.v("dve", "tensor_scalar", [lk, mk8], [mask.key], out=mask.ap, in0=self.logits[:, t, :],
               scalar1=self.max8[:, t, 3:4], scalar2=None, op0=ALU.is_ge)
        b2 = self.nb()
        self.mm(ps[:, b2, 0:NE], [(self.ustrict[:], mask.ap)], [mask.key, "ustrict"], psk(b2, h=0))
        self.mm(ps[:, b2, 256:256 + NE], [(self.ones_f[:], mask.ap)], [mask.key, "ones_f"], psk(b2, h=1))
        self.v("dve", "tensor_tensor", psk(b2, h=0) + ["run_bc"], [("pos", t)], out=self.pos[:, t, :], in0=ps[:, b2, 0:NE],
               in1=self.run_bc[:, :], op=ALU.add)
        self.v("dve", "tensor_tensor", psk(b2, h=1) + ["run_bc"], ["run_bc"], out=self.run_bc[:, :], in0=ps[:, b2, 256:256 + NE],
               in1=self.run_bc[:, :], op=ALU.add)

    def route_finalize(self, l=0):
        t1 = self.abuf("rf_t1", [NE]); m = self.abuf("rf_m", [NE]); padded = self.abuf("rf_pad", [NE])
        cmp = self.abuf("rf_cmp", [NBLK, NE]); wf = self.abuf("rf_wf", [NBLK, 8])
        ti = m.ap.bitcast(I32)
        self.v("dve", "tensor_scalar", ["run_bc"], [m.key], out=ti, in0=self.run_bc[:, :], scalar1=float(BLK - 1), scalar2=None, op0=ALU.add)
        self.v("dve", "tensor_scalar", [m.key], [m.key], out=ti, in0=ti, scalar1=8, scalar2=8, op0=ALU.arith_shift_right, op1=ALU.logical_shift_left)
        self.v("dve", "tensor_copy", [m.key], [padded.key], out=padded.ap, in_=ti)
        self.v("dve", "tensor_tensor_scan", [padded.key, "ones32"], ["pad_ends"], out=self.pad_ends[:, :], data0=self.ones32[:, :],
               data1=padded.ap, initial=0.0, op0=ALU.mult, op1=ALU.add)
        self.v("dve", "tensor_tensor", ["pad_ends", padded.key], ["pad_starts"], out=self.pad_starts[:, :], in0=self.pad_ends[:, :],
               in1=padded.ap, op=ALU.subtract)
        pe_b = self.pad_ends[:, :].unsqueeze(1).to_broadcast([128, NBLK, NE])
        th_b = self.thr[:, :].unsqueeze(2).to_broadcast([128, NBLK, NE])
        self.v("dve", "tensor_tensor", ["pad_ends", "thr"], [cmp.key], out=cmp.ap, in0=pe_b, in1=th_b, op=ALU.is_le)
        self.v("dve", "tensor_reduce", [cmp.key], ["be"], out=self.be[:, :], in_=cmp.ap, axis=AX.X, op=ALU.add)
        self.v("dve", "tensor_scalar", ["be"], ["be"], out=self.be[:, :], in0=self.be[:, :], scalar1=float(NE - 1), scalar2=None, op0=ALU.min)
        be_b = self.be[:, :].unsqueeze(2).to_broadcast([128, NBLK, 8])
        cb_b = self.cbase[:, :].unsqueeze(1).to_broadcast([128, NBLK, 8])
        self.v("dve", "scalar_tensor_tensor", ["be", "cbase"], [wf.key], out=wf.ap, in0=be_b, scalar=1024.0, in1=cb_b, op0=ALU.mult, op1=ALU.add)
        if l:
            self.v("dve", "tensor_scalar", [wf.key], [wf.key], out=wf.ap, in0=wf.ap, scalar1=float(l * NE * D), scalar2=None, op0=ALU.add)
        self.v("dve", "tensor_copy", [wf.key], ["widx_i"], out=self.widx_i[:, :, :], in_=wf.ap)

    def scatter_tmp(self, tag):
        d = dict(xf=self.abuf("sc_xf" + tag, [D]), xb=self.abuf("sc_xb" + tag, [D], BF16),
                 destfull=self.abuf("sc_df" + tag, [NE]), oh=self.abuf("sc_oh" + tag, [4, NE]), junk=self.abuf("sc_junk" + tag, [NE]),
                 destf=self.abuf("sc_destf" + tag, [4]), ge=self.abuf("sc_ge" + tag, [4]), gsum=self.abuf("sc_gsum" + tag, [1]),
                 negm=self.abuf("sc_negm" + tag, [1]), gd=self.abuf("sc_gd" + tag, [NE]))
        return d

    def scatter_tile(self, t, xres, xrows, tmp, do_scatter=True):
        ps = self.ps
        xb, xf = tmp["xb"], tmp["xf"]
        destfull, oh, junk, destf = tmp["destfull"], tmp["oh"], tmp["junk"], tmp["destf"]
        ge, gsum, negm, gd = tmp["ge"], tmp["gsum"], tmp["negm"], tmp["gd"]
        self.dma(xf.ap, xres[t * 128:(t + 1) * 128, :], [("xres", t)], [xf.key])
        self.v("act", "activation", [xf.key], [xb.key], out=xb.ap, in_=xf.ap, func=AF.Copy)
        self.v("dve", "tensor_tensor", [("pos", t), "pad_starts"], [destfull.key], out=destfull.ap, in0=self.pos[:, t, :], in1=self.pad_starts[:, :], op=ALU.add)
        self.v("dve", "tensor_scalar", [("max8", t)], [negm.key], out=negm.ap, in0=self.max8[:, t, 0:1], scalar1=-1.0, scalar2=None, op0=ALU.mult)
        self.v("act", "activation", [("max8", t), negm.key], [ge.key, gsum.key], out=ge.ap, in_=self.max8[:, t, 0:4], func=AF.Exp,
               bias=negm.ap[:, 0:1], scale=1.0, accum_out=gsum.ap)
        self.v("dve", "reciprocal", [gsum.key], [gsum.key], out=gsum.ap, in_=gsum.ap)
        self.v("dve", "tensor_scalar", [ge.key, gsum.key], [("gates", t)], out=self.gates[:, t, :], in0=ge.ap, scalar1=gsum.ap[:, 0:1], scalar2=None, op0=ALU.mult)
        for j in range(4):
            self.v("dve", "tensor_scalar", [("logits", t), ("max8", t)], [(oh.key, j)], out=oh.ap[:, j, :], in0=self.logits[:, t, :],
                   scalar1=self.max8[:, t, j:j + 1], scalar2=None, op0=ALU.is_equal)
            self.v("dve", "scalar_tensor_tensor", [(oh.key, j), destfull.key], [(destf.key, j), junk.key], out=junk.ap, in0=oh.ap[:, j, :], scalar=1.0, in1=destfull.ap,
                   op0=ALU.mult, op1=ALU.mult, accum_out=destf.ap[:, j:j + 1])
        self.v("dve", "tensor_copy", [(destf.key, j) for j in range(4)], [("dest_i", t)], out=self.dest_i[:, t, :], in_=destf.ap)
        for j in range(4 if do_scatter else 0):
            self.P.op("pool", lambda e, j=j: e.indirect_dma_start(
                out=xrows[:, :], out_offset=bass.IndirectOffsetOnAxis(ap=self.dest_i[:, t, j:j + 1], axis=0),
                in_=xb.ap, in_offset=None), reads=[xb.key, ("dest_i", t)], writes=[("xrows_sc", t, j)], dma=True)

    def moe_blocks(self, l, W, xrows, yrows, nblk=NBLK, sub='z'):
        ps = self.ps
        psb = ps[:, :, :].bitcast(BF16)
        ab = self.abuf
        wgu = [ab("wgu%d" % s, [8, 2048], BF16) for s in range(2)]
        wd = [ab("wd%d" % s, [8, D], BF16) for s in range(2)]
        xbt = [ab("xbt%d" % s, [2, D], BF16) for s in range(2)]
        xT = [ab("xTb%d" % s, [8, BLK], BF16) for s in range(2)]
        actT = [ab("actT%d" % s, [8, BLK], BF16) for s in range(2)]
        ysb1 = ab("ysb", [2, D], F32)
        ysb = [ysb1, ysb1]
        ohb = [ab("ohb%d" % s, [BLK], F32) for s in range(2)]
        gt = [ab("gt%d" % s, [BLK], F32) for s in range(2)]
        st = [ab("st%d" % s, [BLK], F32) for s in range(2)]
        ut = [ab("ut%d" % s, [BLK], F32) for s in range(2)]
        gst = [ab("gst%d" % s, [BLK], F32) for s in range(2)]
        wgu_d = W["moe_w_gu"].rearrange("l e k f -> (l e k) f")
        wd_d = W["moe_w_down"].rearrange("l e k f -> (l e k) f")
        scat_keys = [("xrows_sc", t, j) for t in range(NT) for j in range(4)]
        for b in range(nblk):
            s = b % 2
            for c in range(8):
                self.P.op("pool", lambda e, c=c, s=s, b=b: e.indirect_dma_start(
                    out=wgu[s].ap[:, c, :], out_offset=None, in_=wgu_d,
                    in_offset=bass.IndirectOffsetOnAxis(ap=self.widx_i[:, b, c:c + 1], axis=0)),
                    reads=["widx_i"], writes=[(wgu[s].key, c)], dma=True)
            for c in range(8):
                self.P.op("pool", lambda e, c=c, s=s, b=b: e.indirect_dma_start(
                    out=wd[s].ap[:, c, :], out_offset=None, in_=wd_d,
                    in_offset=bass.IndirectOffsetOnAxis(ap=self.widx_i[:, b, c:c + 1], axis=0)),
                    reads=["widx_i"], writes=[(wd[s].key, c)], dma=True)
            self.dma(xbt[s].ap, xrows[b * BLK:(b + 1) * BLK, :].rearrange("(i p) d -> p i d", p=128),
                     scat_keys if b < 2 else [], [xbt[s].key])
            if sub < 'b':
                continue
            for i in range(2):
                for half in range(2):
                    bk = self.nb()
                    for q in range(4):
                        kc = half * 4 + q
                        self.tr(psb[:, bk, q * 128:(q + 1) * 128], xbt[s].ap[:, i, kc * 128:(kc + 1) * 128], self.ident_b[:],
                                [xbt[s].key, "ident_b"], psk(bk, q))
                    o_ = xT[s].ap[:, half * 4:(half + 1) * 4, i * 128:(i + 1) * 128]
                    i_ = psb[:, bk, 0:512].rearrange("p (a c) -> p a c", a=4)
                    if half:
                        self.v("act", "activation", psk(bk), [(xT[s].key, i, half)], out=o_, in_=i_, func=AF.Copy)
                    else:
                        self.v("dve", "tensor_copy", psk(bk), [(xT[s].key, i, half)], out=o_, in_=i_)
            self.v("dve", "tensor_scalar", ["be", "iota_p"], [ohb[s].key], out=ohb[s].ap[0:NE, :], in0=self.iota_p[0:NE, :],
                   scalar1=self.be[0:NE, b:b + 1], scalar2=None, op0=ALU.is_equal)
            xT_keys = [(xT[s].key, i, h) for i in range(2) for h in range(2)]
            if sub < 'c':
                continue
            for j in range(8):
                bk = self.nb()
                for gu in range(2):
                    pairs = [(wgu[s].ap[:, kc, j * 256 + gu:(j + 1) * 256:2], xT[s].ap[:, kc, :]) for kc in range(8)]
                    pairs.append((self.bgu[:, j * 256 + gu:(j + 1) * 256:2], ohb[s].ap[0:NE, :]))
                    self.mm(ps[:, bk, gu * BLK:(gu + 1) * BLK], pairs,
                            xT_keys + [(wgu[s].key, c) for c in range(8)] + ["bgu", ohb[s].key], psk(bk, h=gu))
                if sub < 'd':
                    continue
                self.v("dve", "tensor_scalar", psk(bk, h=0), [gt[s].key], out=gt[s].ap, in0=ps[:, bk, 0:BLK], scalar1=7.0, scalar2=None, op0=ALU.min)
                self.v("act", "activation", [gt[s].key], [st[s].key], out=st[s].ap, in_=gt[s].ap, func=AF.Sigmoid, scale=1.702)
                self.v("dve", "tensor_scalar", psk(bk, h=1), [ut[s].key], out=ut[s].ap, in0=ps[:, bk, BLK:2 * BLK], scalar1=7.0, scalar2=-7.0, op0=ALU.min, op1=ALU.max)
                self.v("dve", "tensor_tensor", [gt[s].key, st[s].key], [gst[s].key], out=gst[s].ap, in0=gt[s].ap, in1=st[s].ap, op=ALU.mult)
                self.v("dve", "scalar_tensor_tensor", [ut[s].key, gst[s].key], [(actT[s].key, j)], out=actT[s].ap[:, j, :], in0=ut[s].ap, scalar=1.0, in1=gst[s].ap,
                       op0=ALU.add, op1=ALU.mult)
            if sub < 'e':
                continue
            for rt in range(2):
                for dh in range(2):
                    bk = self.nb()
                    pairs = [(actT[s].ap[:, j, rt * 128:(rt + 1) * 128], wd[s].ap[:, j, dh * 512:(dh + 1) * 512]) for j in range(8)]
                    pairs.append((ohb[s].ap[0:NE, rt * 128:(rt + 1) * 128], self.bdn[:, dh * 512:(dh + 1) * 512]))
                    self.mm(ps[:, bk, :], pairs, [(actT[s].key, j) for j in range(8)] + [(wd[s].key, c) for c in range(8)] + ["bdn", ohb[s].key], psk(bk))
                    self.v("act", "activation", psk(bk), [(ysb[s].key, rt, dh)], out=ysb[s].ap[:, rt, dh * 512:(dh + 1) * 512], in_=ps[:, bk, :], func=AF.Copy)
            self.dma(yrows[b * BLK:(b + 1) * BLK, :].rearrange("(i p) d -> p i d", p=128), ysb[s].ap,
                     [(ysb[s].key, rt, dh) for rt in range(2) for dh in range(2)], [("yrows", b)])
        self.yrow_keys = [("yrows", b) for b in range(nblk)]

    def ln2_phase(self, l, xres, yrows, out_dram, XT=None):
        ps = self.ps
        psb = ps[:, :, :].bitcast(BF16)
        self.load_ln(l, 1)
        tm = []
        for s in range(2):
            tm.append(dict(xf=self.abuf("l2_xf%d" % s, [D]), yg=[self.abuf("l2_yg%d_%d" % (s, j), [D]) for j in range(4)],
                           xn=self.abuf("l2_xn%d" % s, [D]), xb=self.abuf("l2_xb%d" % s, [D], BF16), ln=self.ln_tmp("l2_%d" % s)))
        for t in range(NT):
            d = tm[t % 2]
            xf, yg, xn, xb = d["xf"], d["yg"], d["xn"], d["xb"]
            self.dma(xf.ap, xres[t * 128:(t + 1) * 128, :], [("xres", t)], [xf.key])
            for j in range(4):
                self.P.op("pool", lambda e, j=j, yg=yg, t=t: e.indirect_dma_start(
                    out=yg[j].ap, out_offset=None, in_=yrows[:, :],
                    in_offset=bass.IndirectOffsetOnAxis(ap=self.dest_i[:, t, j:j + 1], axis=0)),
                    reads=self.yrow_keys + [("dest_i", t)], writes=[yg[j].key], dma=True)
            self.v("act", "activation", [xf.key], [xf.key], out=xf.ap, in_=xf.ap, func=AF.Copy, scale=ALPHA)
            for j in range(4):
                self.v("dve", "scalar_tensor_tensor", [yg[j].key, ("gates", t), xf.key], [xf.key],
                       out=xf.ap, in0=yg[j].ap, scalar=self.gates[:, t, j:j + 1], in1=xf.ap, op0=ALU.mult, op1=ALU.add)
            self.ln_tile(xf, 1, xn, d["ln"])
            self.dma(out_dram[t * 128:(t + 1) * 128, :], xn.ap, [xn.key], [("xres", t)])
            if XT is not None:
                self.v("act", "activation", [xn.key], [xb.key], out=xb.ap, in_=xn.ap, func=AF.Copy)
                bk = self.nb()
                for kc in range(8):
                    self.tr(psb[:, bk, kc * 128:(kc + 1) * 128], xb.ap[:, kc * 128:(kc + 1) * 128], self.ident_b[:], [xb.key, "ident_b"], psk(bk, kc // 2))
                self.v("dve", "tensor_copy", psk(bk), [("XT", t)], out=XT[:, :, t * 128:(t + 1) * 128],
                       in_=psb[:, bk, :].rearrange("p (a c) -> p a c", a=8))

    def post_mixer_phase(self, l, x_src, xres, h_dram=None, h_fn=None):
        self.load_ln(l, 0)
        tm = []
        for s in range(2):
            tm.append(dict(xf=self.abuf("pm_xf%d" % s, [D]), hf=self.abuf("pm_hf%d" % s, [D]), xn=self.abuf("pm_xn%d" % s, [D]),
                           xT=self.abuf("pm_xT%d" % s, [8, 128]), mask=self.abuf("pm_mask%d" % s, [NE]), ln=self.ln_tmp("pm_%d" % s)))
        for t in range(NT):
            d = tm[t % 2]
            xf, hf, xn = d["xf"], d["hf"], d["xn"]
            self.dma(xf.ap, x_src[t * 128:(t + 1) * 128, :], [("xres", t)], [xf.key])
            if h_dram is not None:
                self.dma(hf.ap, h_dram[t * 128:(t + 1) * 128, :], [], [hf.key])
                self.v("dve", "scalar_tensor_tensor", [xf.key, hf.key], [xf.key], out=xf.ap, in0=xf.ap, scalar=ALPHA, in1=hf.ap, op0=ALU.mult, op1=ALU.add)
            else:
                for dh, (hap, hkeys) in enumerate(h_fn(t)):
                    self.v("dve", "scalar_tensor_tensor", [xf.key] + hkeys, [xf.key], out=xf.ap[:, dh * 512:(dh + 1) * 512],
                           in0=xf.ap[:, dh * 512:(dh + 1) * 512], scalar=ALPHA, in1=hap, op0=ALU.mult, op1=ALU.add)
            self.ln_tile(xf, 0, xn, d["ln"])
            self.dma(xres[t * 128:(t + 1) * 128, :], xn.ap, [xn.key], [("xres", t)])
            self.route_tile(t, xn, d["xT"], d["mask"])

    def moe_layer(self, l, W, xres, xrows, yrows, out_dram, XT=None, nblk=NBLK, stage=9, xt_words=0):
        self.A.reset()
        self.P.barrier()
        if stage < 2:
            return
        self.route_finalize(l)
        tm = [self.scatter_tmp(str(s)) for s in range(2)]
        for t in range(NT):
            self.scatter_tile(t, xres, xrows, tm[t % 2], do_scatter=(stage >= 3))
        self.A.reset()
        self.P.barrier()
        if stage < 4:
            return
        self.moe_blocks(l, W, xrows, yrows, nblk=nblk, sub=self.sub)
        self.A.reset()
        self.P.barrier()
        if stage < 5:
            return
        if XT is not None:
            self.A.words -= xt_words
        self.ln2_phase(l, xres, yrows, out_dram, XT=XT)
        if XT is not None:
            self.A.words += xt_words
        self.P.barrier()


class FullBuilder(Builder, MoEMixin):
    sub = 'z'


WNAMES = ["gdn_w_in", "gdn_conv", "gdn_a_log", "gdn_dt_bias", "gdn_norm_g", "gdn_w_out", "sb_w_qkv", "sb_w_out",
          "ln1_g", "ln1_b", "moe_w_router", "moe_b_router", "moe_w_gu", "moe_b_gu", "moe_w_down", "moe_b_down", "ln2_g", "ln2_b"]


SB_SCALE = 64 ** -0.5


class SBMixin:
    def sb_consts(self):
        self.negones = self.sb("negones", [128, 128], F32)
        self.tri_neg = self.sb("tri_neg", [128, 128], F32)
        self.v("pool", "memset", [], ["negones"], ap=self.negones[:], constant=-1.0)
        self.v("pool", "affine_select", ["negones"], ["tri_neg"], out=self.tri_neg[:], in_=self.negones[:],
               pattern=[[-1, 128]], compare_op=ALU.is_ge, fill=0.0, base=0, channel_multiplier=1)

    def cast_load(self, dst_ap, src_ap, reads, writes):
        self.P.op("pool", lambda e: e.dma_start(out=dst_ap, in_=src_ap), reads=reads, writes=writes, dma=True)

    def sb_proj(self, j, W, XT, qT_d, kT_d, v_d):
        ps = self.ps
        wq = self.abuf("sb_wq", [8, 3072], BF16)
        w_d = W["sb_w_qkv"][j]
        for sec in range(3):
            self.cast_load(wq.ap[:, :, sec * 1024:(sec + 1) * 1024],
                           w_d[:, sec * 1024:(sec + 1) * 1024].rearrange("(kc p) f -> p kc f", p=128), [], [(wq.key, sec)])
        stg = [self.abuf("sb_stg%d" % s, [T], BF16) for s in range(2)]
        xt_keys = [("XT", t) for t in range(NT)]
        n = 0
        for sec, dst in ((0, qT_d), (1, kT_d)):
            for c in range(8):
                s = n % 2; n += 1
                for tb in range(8):
                    bk = self.nb()
                    pairs = [(wq.ap[:, kc, sec * 1024 + c * 128: sec * 1024 + (c + 1) * 128], XT[:, kc, tb * 512:(tb + 1) * 512]) for kc in range(8)]
                    self.mm(ps[:, bk, :], pairs, xt_keys + [(wq.key, sec)], psk(bk))
                    eng = "act" if tb % 2 else "dve"
                    if eng == "act":
                        self.v("act", "activation", psk(bk), [(stg[s].key, tb)], out=stg[s].ap[:, tb * 512:(tb + 1) * 512], in_=ps[:, bk, :], func=AF.Copy)
                    else:
                        self.v("dve", "tensor_copy", psk(bk), [(stg[s].key, tb)], out=stg[s].ap[:, tb * 512:(tb + 1) * 512], in_=ps[:, bk, :])
                self.dma(dst[c * 128:(c + 1) * 128, :], stg[s].ap, [(stg[s].key, tb) for tb in range(8)], [("sbqk", sec, c)])
        vst = [self.abuf("sb_vst%d" % s, [D], BF16) for s in range(2)]
        for t in range(NT):
            s = t % 2
            for half in range(2):
                bk = self.nb()
                pairs = [(XT[:, kc, t * 128:(t + 1) * 128], wq.ap[:, kc, 2048 + half * 512: 2048 + (half + 1) * 512]) for kc in range(8)]
                self.mm(ps[:, bk, :], pairs, xt_keys + [(wq.key, 2)], psk(bk))
                if half:
                    self.v("act", "activation", psk(bk), [(vst[s].key, half)], out=vst[s].ap[:, half * 512:(half + 1) * 512], in_=ps[:, bk, :], func=AF.Copy)
                else:
                    self.v("dve", "tensor_copy", psk(bk), [(vst[s].key, half)], out=vst[s].ap[:, half * 512:(half + 1) * 512], in_=ps[:, bk, :])
            self.dma(v_d[t * 128:(t + 1) * 128, :], vst[s].ap, [(vst[s].key, 0), (vst[s].key, 1)], [("sbv", t)])

    def sb_attn(self, qT_d, kT_d, v_d, OT, nchunks=8, nqb=8):
        ps = self.ps
        ab = self.abuf
        qT = ab("sb_qT", [T], BF16); kT = ab("sb_kT", [T], BF16)
        vp = [ab("sb_vp%d" % hh, [NT, 128], BF16) for hh in range(2)]
        e_t = [ab("sb_e%d" % s, [512]) for s in range(2)]
        sp_t = [ab("sb_sp%d" % s, [512]) for s in range(2)]
        ec_t = [ab("sb_ec%d" % s, [512]) for s in range(2)]
        w32 = [ab("sb_w32%d" % s, [512]) for s in range(2)]
        w_t = [ab("sb_w%d" % s, [512], BF16) for s in range(2)]
        lsum = [ab("sb_ls%d" % s, [512]) for s in range(2)]
        for hh in range(2):
            self.v("pool", "memset", [], [vp[hh].key], ap=vp[hh].ap, constant=0.0)
        nq = 0
        for c in range(nchunks):
            self.dma(qT.ap, qT_d[c * 128:(c + 1) * 128, :], [("sbqk", 0, c)], [qT.key])
            self.dma(kT.ap, kT_d[c * 128:(c + 1) * 128, :], [("sbqk", 1, c)], [kT.key])
            for hh in range(2):
                self.dma(vp[hh].ap[:, :, hh * 64:(hh + 1) * 64],
                         v_d[:, c * 128 + hh * 64: c * 128 + (hh + 1) * 64].rearrange("(t p) d -> p t d", p=128),
                         [("sbv", t) for t in range(NT)], [vp[hh].key], allow_slow_non_contiguous=False)
            for hh in range(2):
                p0, p1 = hh * 64, (hh + 1) * 64
                for qb in range(nqb):
                    ls = lsum[nq % 2]; nq += 1
                    ob = self.nb()
                    okey = ("sb_oacc", ob)
                    nkb = 4 * qb + 4
                    for i, kb in enumerate(range(nkb - 1, -1, -1)):
                        s = i % 2
                        r = kb - 4 * qb
                        zb = self.nb()
                        if zb == ob:
                            zb = self.nb()
                        self.mm(ps[:, zb, :], [(kT.ap[p0:p1, kb * 128:(kb + 1) * 128], qT.ap[p0:p1, qb * 512:(qb + 1) * 512])],
                                [qT.key, kT.key], psk(zb))
                        self.v("act", "activation", psk(zb), [e_t[s].key], out=e_t[s].ap, in_=ps[:, zb, :], func=AF.Exp, scale=SB_SCALE)
                        self.v("act", "activation", [e_t[s].key], [sp_t[s].key], out=sp_t[s].ap, in_=e_t[s].ap, func=AF.Ln, bias=1.0, scale=1.0)
                        if r >= 0:
                            self.v("pool", "affine_select", [sp_t[s].key], [sp_t[s].key], out=sp_t[s].ap, in_=sp_t[s].ap,
                                   pattern=[[1, 512]], compare_op=ALU.is_gt, fill=0.0, base=-128 * r, channel_multiplier=-1)
                        cb = self.nb()
                        if cb == ob:
                            cb = self.nb()
                        pairs = [(self.tri_neg[:], sp_t[s].ap)]
                        rd = ["tri_neg", sp_t[s].key]
                        if i > 0:
                            pairs.append((self.negones[:], ls.ap))
                            rd += ["negones", ls.key]
                        self.mm(ps[:, cb, :], pairs, rd, psk(cb))
                        self.v("act", "activation", psk(cb), [ec_t[s].key], out=ec_t[s].ap, in_=ps[:, cb, :], func=AF.Exp)
                        if i == 0:
                            self.v("pool", "tensor_copy", [sp_t[s].key], [ls.key], out=ls.ap, in_=sp_t[s].ap)
                        elif i < nkb - 1:
                            self.v("pool", "tensor_tensor", [sp_t[s].key, ls.key], [ls.key], out=ls.ap, in0=ls.ap, in1=sp_t[s].ap, op=ALU.add)
                        if r >= 0:
                            self.v("dve", "tensor_tensor", [e_t[s].key, ec_t[s].key], [w32[s].key], out=w32[s].ap, in0=e_t[s].ap, in1=ec_t[s].ap, op=ALU.mult)
                            self.v("pool", "affine_select", [w32[s].key], [w_t[s].key], out=w_t[s].ap, in_=w32[s].ap,
                                   pattern=[[1, 512]], compare_op=ALU.is_gt, fill=0.0, base=-128 * r, channel_multiplier=-1)
                        else:
                            self.v("dve", "tensor_tensor", [e_t[s].key, ec_t[s].key], [w_t[s].key], out=w_t[s].ap, in0=e_t[s].ap, in1=ec_t[s].ap, op=ALU.mult)

                        def omm(e, kb=kb, s=s, i=i, nkb=nkb, ob=ob, hh=hh):
                            return e.matmul(ps[:, ob, :], lhsT=vp[hh].ap[:, kb, :], rhs=w_t[s].ap, start=(i == 0), stop=(i == nkb - 1))
                        self.P.op("pe", omm, reads=[vp[hh].key, w_t[s].key], writes=[okey] if i < nkb - 1 else [("ps", ob)])
                    self.v("act", "activation", [("ps", ob), okey], [("OT", c, hh, qb)], out=OT[p0:p1, c, qb * 512:(qb + 1) * 512], in_=ps[p0:p1, ob, :], func=AF.Copy)

    def sb_outproj_hfn(self, j, W, OT, ot_keys):
        ps = self.ps
        wo = self.abuf("sb_wo", [8, D], BF16)
        self.cast_load(wo.ap, W["sb_w_out"][j].rearrange("(kc p) f -> p kc f", p=128), [], [wo.key])

        def h_fn(t):
            res = []
            for dh in range(2):
                bk = self.nb()
                pairs = [(OT[:, kc, t * 128:(t + 1) * 128], wo.ap[:, kc, dh * 512:(dh + 1) * 512]) for kc in range(8)]
                self.mm(ps[:, bk, :], pairs, ot_keys + [wo.key], psk(bk))
                res.append((ps[:, bk, :], psk(bk)))
            return res
        return h_fn


class FullBuilder2(Builder, MoEMixin, SBMixin):
    sub = 'z'


XT_WORDS = 8 * T // 2


def xt_view(B):
    off = B.A.words - XT_WORDS
    v = B.arena_t[:, off:off + XT_WORDS].bitcast(BF16)
    return v.rearrange("p (a b) -> p a b", a=8)


def xt_from_x(B, x_src, XT):
    ps = B.ps
    psb = ps[:, :, :].bitcast(BF16)
    xf = [B.abuf("x0_xf%d" % s, [D]) for s in range(2)]
    xb = [B.abuf("x0_xb%d" % s, [D], BF16) for s in range(2)]
    for t in range(NT):
        s = t % 2
        B.dma(xf[s].ap, x_src[t * 128:(t + 1) * 128, :], [("xres", t)], [xf[s].key])
        B.v("act", "activation", [xf[s].key], [xb[s].key], out=xb[s].ap, in_=xf[s].ap, func=AF.Copy)
        bk = B.nb()
        for kc in range(8):
            B.tr(psb[:, bk, kc * 128:(kc + 1) * 128], xb[s].ap[:, kc * 128:(kc + 1) * 128], B.ident_b[:], [xb[s].key, "ident_b"], psk(bk))
        B.v("dve", "tensor_copy", psk(bk), [("XT", t)], out=XT[:, :, t * 128:(t + 1) * 128],
            in_=psb[:, bk, :].rearrange("p (a c) -> p a c", a=8))


NVH = 16


class GDNMixin:
    def gdn_consts(self):
        mk = {}
        for name in ("g_same", "g_triinc", "g_mu", "g_mus", "g_tmp"):
            mk[name] = self.sb(name, [128, 128], F32)
        self.g_same, self.g_triinc, self.g_mu, self.g_mus = mk["g_same"], mk["g_triinc"], mk["g_mu"], mk["g_mus"]
        tmp = mk["g_tmp"]
        self.v("pool", "memset", [], ["g_same"], ap=self.g_same[:], constant=0.0)
        self.v("pool", "memset", ["g_same"], ["g_same"], ap=self.g_same[0:64, 0:64], constant=1.0)
        self.v("pool", "memset", ["g_same"], ["g_same"], ap=self.g_same[64:128, 64:128], constant=1.0)
        self.v("pool", "affine_select", ["g_same"], ["g_triinc"], out=self.g_triinc[:], in_=self.g_same[:],
               pattern=[[1, 128]], compare_op=ALU.is_ge, fill=0.0, base=0, channel_multiplier=-1)
        self.v("pool", "tensor_copy", ["g_triinc"], ["g_mu"], out=self.g_mu[:], in_=self.g_triinc[:])
        self.v("pool", "affine_select", ["g_same"], ["g_mus"], out=self.g_mus[:], in_=self.g_same[:],
               pattern=[[1, 128]], compare_op=ALU.is_gt, fill=0.0, base=0, channel_multiplier=-1)
        self.g_sel = self.sb("g_sel", [NVH, NVH, 128], F32)
        self.v("pool", "memset", [], ["g_sel"], ap=self.g_sel[:], constant=1.0)
        self.v("pool", "affine_select", ["g_sel"], ["g_sel"], out=self.g_sel[:], in_=self.g_sel[:],
               pattern=[[-1, NVH], [0, 128]], compare_op=ALU.is_equal, fill=0.0, base=0, channel_multiplier=1)
        self.g_beta = self.sb("g_beta", [128, NT, NVH], F32)
        self.g_gcum = self.sb("g_gcum", [128, NT, NVH], F32)
        self.g_bexp = self.sb("g_bexp", [128, NT, NVH], F32)
        self.g_kdsc = self.sb("g_kdsc", [128, NT, NVH], F32)

    def gdn_g1(self, j, W, XT, qkvT_g, zs_g):
        ps = self.ps
        ab = self.abuf
        xt_keys = [("XT", t) for t in range(NT)]
        w_d = W["gdn_w_in"][j]
        H2 = T // 2
        cw = ab("g_cw", [32, 4])
        for cc in range(32):
            self.dma(cw.ap[:, cc, :], W["gdn_conv"][j][:, cc * 128:(cc + 1) * 128].rearrange("k c -> c k"), [], [(cw.key, cc)],
                     allow_slow_non_contiguous=True)
        wsec1 = ab("g_wsec", [8, 1024], BF16)
        wsec = [wsec1, wsec1]
        pc = [ab("g_pc%d" % s, [3 + H2]) for s in range(2)]
        acc = [ab("g_acc%d" % s, [H2]) for s in range(2)]
        sq = ab("g_sq", [H2])
        rs = [ab("g_rs%d" % s, [512]) for s in range(2)]
        n = 0
        for sec in range(4):
            ws = wsec[sec % 2]
            self.cast_load(ws.ap, w_d[:, sec * 1024:(sec + 1) * 1024].rearrange("(kc p) f -> p kc f", p=128), [], [ws.key])
            for c8 in range(8):
                cc = sec * 8 + c8
                for half in range(2):
                    p_ = pc[n % 2]; po = pc[(n + 1) % 2]; a_ = acc[n % 2]; n += 1
                    if half == 0:
                        self.v("pool", "memset", [], [(p_.key, "halo")], ap=p_.ap[:, 0:3], constant=0.0)
                    else:
                        self.v("pool", "tensor_copy", [(po.key, 3)], [(p_.key, "halo")], out=p_.ap[:, 0:3], in_=po.ap[:, H2:H2 + 3])
                    for tb in range(4):
                        bk = self.nb()
                        t0 = half * H2 + tb * 512
                        pairs = [(ws.ap[:, kc, c8 * 128:(c8 + 1) * 128], XT[:, kc, t0:t0 + 512]) for kc in range(8)]
                        self.mm(ps[:, bk, :], pairs, xt_keys + [ws.key], psk(bk))
                        self.v("act", "activation", psk(bk), [(p_.key, tb)], out=p_.ap[:, 3 + tb * 512:3 + (tb + 1) * 512], in_=ps[:, bk, :], func=AF.Copy)
                    pk = [(p_.key, tb) for tb in range(4)] + [(p_.key, "halo")]
                    self.v("dve", "tensor_scalar", pk + [(cw.key, cc)], [a_.key], out=a_.ap, in0=p_.ap[:, 3:3 + H2], scalar1=cw.ap[:, cc, 3:4], scalar2=None, op0=ALU.mult)
                    for tap in (2, 1, 0):
                        self.v("dve", "scalar_tensor_tensor", pk + [(cw.key, cc), a_.key], [a_.key], out=a_.ap, in0=p_.ap[:, tap:tap + H2],
                               scalar=cw.ap[:, cc, tap:tap + 1], in1=a_.ap, op0=ALU.mult, op1=ALU.add)
                    self.v("act", "activation", [a_.key], [a_.key], out=a_.ap, in_=a_.ap, func=AF.Silu)
                    if sec < 2:
                        self.v("pool", "tensor_tensor", [a_.key], [sq.key], out=sq.ap, in0=a_.ap, in1=a_.ap, op=ALU.mult)
                        for tb in range(4):
                            bk = self.nb()
                            r_ = rs[tb % 2]
                            self.mm(ps[:, bk, :], [(self.ones_f[:], sq.ap[:, tb * 512:(tb + 1) * 512])], [sq.key, "ones_f"], psk(bk))
                            if sec == 0:
                                self.v("act", "activation", psk(bk), [r_.key], out=r_.ap, in_=ps[:, bk, :], func=AF.Sqrt, scale=128.0, bias=128.0e-6)
                            else:
                                self.v("act", "activation", psk(bk), [r_.key], out=r_.ap, in_=ps[:, bk, :], func=AF.Sqrt, scale=1.0, bias=1.0e-6)
                            self.v("dve", "reciprocal", [r_.key], [r_.key], out=r_.ap, in_=r_.ap)
                            self.v("dve", "tensor_tensor", [r_.key, a_.key], [a_.key], out=a_.ap[:, tb * 512:(tb + 1) * 512], in0=a_.ap[:, tb * 512:(tb + 1) * 512], in1=r_.ap, op=ALU.mult)
                    self.dma(qkvT_g[cc * 128:(cc + 1) * 128, half * H2:(half + 1) * H2], a_.ap, [a_.key], [("qkvT", cc, half)])
        zst = acc
        for zsec in range(2):
            ws = wsec[zsec % 2]
            self.cast_load(ws.ap, w_d[:, 4096 + zsec * 1024:4096 + (zsec + 1) * 1024].rearrange("(kc p) f -> p kc f", p=128), [], [ws.key])
            for t in range(NT):
                z_ = zst[t % 2]
                for blk in range(2):
                    bk = self.nb()
                    pairs = [(XT[:, kc, t * 128:(t + 1) * 128], ws.ap[:, kc, blk * 512:(blk + 1) * 512]) for kc in range(8)]
                    self.mm(ps[:, bk, :], pairs, xt_keys + [ws.key], psk(bk))
                    self.v("act", "activation", psk(bk), [z_.key], out=z_.ap[:, blk * 512:(blk + 1) * 512], in_=ps[:, bk, :], func=AF.Silu)
                self.dma(zs_g[t * 128:(t + 1) * 128, zsec * 1024:(zsec + 1) * 1024], z_.ap[:, 0:1024], [z_.key], [("zs", t, zsec)])
        wba = ab("g_wba", [8, 32], BF16)
        self.cast_load(wba.ap, w_d[:, 6144:6176].rearrange("(kc p) f -> p kc f", p=128), [], [wba.key])
        negA = ab("g_negA", [NVH]); dtb = ab("g_dtb", [NVH])
        self.dma(negA.ap.unsqueeze(1), W["gdn_a_log"][j:j + 1, :].partition_broadcast(128), [], [negA.key])
        self.dma(dtb.ap.unsqueeze(1), W["gdn_dt_bias"][j:j + 1, :].partition_broadcast(128), [], [dtb.key])
        self.v("act", "activation", [negA.key], [negA.key], out=negA.ap, in_=negA.ap, func=AF.Exp)
        self.v("dve", "tensor_scalar", [negA.key], [negA.key], out=negA.ap, in0=negA.ap, scalar1=-1.0, scalar2=None, op0=ALU.mult)
        tb_ = [ab("g_tb%d" % s, [NVH]) for s in range(2)]
        tg_ = [ab("g_tg%d" % s, [NVH]) for s in range(2)]
        tl_ = [ab("g_tl%d" % s, [NVH]) for s in range(2)]
        for t in range(NT):
            s = t % 2
            bk = self.nb()
            pairs = [(XT[:, kc, t * 128:(t + 1) * 128], wba.ap[:, kc, :]) for kc in range(8)]
            self.mm(ps[:, bk, 0:32], pairs, xt_keys + [wba.key], psk(bk))
            self.v("act", "activation", psk(bk), [tb_[s].key], out=tb_[s].ap, in_=ps[:, bk, 0:16], func=AF.Exp, scale=-1.0)
            self.v("dve", "tensor_scalar", [tb_[s].key], [tb_[s].key], out=tb_[s].ap, in0=tb_[s].ap, scalar1=1.0, scalar2=None, op0=ALU.add)
            self.v("dve", "reciprocal", [tb_[s].key], [("g_beta", t)], out=self.g_beta[:, t, :], in_=tb_[s].ap)
            self.v("dve", "tensor_tensor", psk(bk) + [dtb.key], [tg_[s].key], out=tg_[s].ap, in0=ps[:, bk, 16:32], in1=dtb.ap, op=ALU.add)
            self.v("act", "activation", [tg_[s].key], [tg_[s].key], out=tg_[s].ap, in_=tg_[s].ap, func=AF.Exp)
            self.v("act", "activation", [tg_[s].key], [tg_[s].key], out=tg_[s].ap, in_=tg_[s].ap, func=AF.Ln, bias=1.0, scale=1.0)
            self.v("dve", "tensor_tensor", [tg_[s].key, negA.key], [tg_[s].key], out=tg_[s].ap, in0=tg_[s].ap, in1=negA.ap, op=ALU.mult)
            b2 = self.nb()
            self.mm(ps[:, b2, 0:16], [(self.g_triinc[:], tg_[s].ap)], [tg_[s].key, "g_triinc"], psk(b2))
            self.mm(ps[:, b2, 16:32], [(self.g_same[:], tg_[s].ap)], [tg_[s].key, "g_same"], psk(b2))
            self.v("act", "activation", psk(b2), [("g_gcum", t)], out=self.g_gcum[:, t, :], in_=ps[:, b2, 0:16], func=AF.Copy)
            self.v("dve", "tensor_tensor", psk(b2) + [("g_gcum", t)], [tl_[s].key], out=tl_[s].ap, in0=ps[:, b2, 16:32], in1=self.g_gcum[:, t, :], op=ALU.subtract)
            self.v("act", "activation", [tl_[s].key], [("g_kdsc", t)], out=self.g_kdsc[:, t, :], in_=tl_[s].ap, func=AF.Exp)
            self.v("act", "activation", [("g_gcum", t)], [tl_[s].key], out=tl_[s].ap, in_=self.g_gcum[:, t, :], func=AF.Exp)
            self.v("dve", "tensor_tensor", [tl_[s].key, ("g_beta", t)], [("g_bexp", t)], out=self.g_bexp[:, t, :], in0=tl_[s].ap, in1=self.g_beta[:, t, :], op=ALU.mult)

    def gdn_g2(self, j, W, qkvT_g, zs_g, oT_g, ntiles=NT):
        ps = self.ps
        psb = ps[:, :, :].bitcast(BF16)
        ab = self.abuf
        I_ = self.ident_f
        ng = ab("g_ng", [128])
        self.dma(ng.ap.unsqueeze(1), W["gdn_norm_g"][j:j + 1, :].partition_broadcast(128), [], [ng.key])
        NS = 4
        sets = []
        for q in range(NS):
            d = {}
            for nm in ("vb", "kbg", "e1", "dec", "at", "AT", "Am", "R", "P0", "P1", "PT0", "PT1"):
                d[nm] = ab("g2_%s%d" % (nm, q), [128])
            sets.append(d)
        ph = []
        for hl in range(8):
            ph.append([{nm: ab("g2_%s_%d_%d" % (nm, hl, par), [128]) for nm in ("kdec", "eg", "attnT", "qdT", "u", "wT")} for par in range(2)])
        vn = [ab("g2_vn%d" % hl, [128]) for hl in range(8)]
        pk = [[{nm: ab("g2_%s_%d_%d" % (nm, kl, par), [128]) for nm in ("qT", "kT")} for par in range(2)] for kl in range(4)]
        pkc = [{nm: ab("g2_%s_%d" % (nm, kl), [128]) for nm in ("kkm", "qkm", "ktm")} for kl in range(4)]
        vT = [[ab("g2_vT_%d_%d" % (hl, par), [128]) for par in range(2)] for hl in range(8)]
        S = ab("g2_S", [8, 128])
        o_tm = ab("g2_o", [8, 128])
        zs = [ab("g2_zs%d" % par, [8, 128]) for par in range(2)]
        sqo = ab("g2_sqo", [8, 128])
        ogb = ab("g2_ogb", [8, 128], BF16)
        ogT = [ab("g2_ogT%d" % par, [8, 128], BF16) for par in range(2)]
        bT = [ab("g2_bT%d" % par, [2, 128]) for par in range(2)]
        ssum = ab("g2_ssum", [8])
        oT_v = oT_g.rearrange("(h p) t -> p h t", p=128)
        for grp in range(2):
            self.v("pool", "memset", [], [(S.key, hl) for hl in range(8)], ap=S.ap, constant=0.0)
            for i in range(ntiles):
                par = i % 2
                tsl = slice(i * 128, (i + 1) * 128)
                for kl in range(4):
                    kh = grp * 4 + kl
                    self.dma(pk[kl][par]["qT"].ap, qkvT_g[kh * 128:(kh + 1) * 128, tsl], [("qkvT", kh, i // 16)], [pk[kl][par]["qT"].key])
                    self.dma(pk[kl][par]["kT"].ap, qkvT_g[(8 + kh) * 128:(9 + kh) * 128, tsl], [("qkvT", 8 + kh, i // 16)], [pk[kl][par]["kT"].key])
                for hl in range(8):
                    h = grp * 8 + hl
                    self.dma(vT[hl][par].ap, qkvT_g[(16 + h) * 128:(17 + h) * 128, tsl], [("qkvT", 16 + h, i // 16)], [vT[hl][par].key])
                self.dma(zs[par].ap, zs_g[tsl, grp * 1024:(grp + 1) * 1024].rearrange("p (h d) -> p h d", h=8), [("zs", i, grp)], [zs[par].key])
                bk = self.nb()
                self.mm(ps[0:NVH, bk, 0:128], [(self.g_beta[:, i, :], I_[:])], [("g_beta", i), "ident_f"], psk(bk))
                self.mm(ps[0:NVH, bk, 128:256], [(self.g_gcum[:, i, :], I_[:])], [("g_gcum", i), "ident_f"], psk(bk))
                self.v("act", "activation", psk(bk), [bT[par].key], out=bT[par].ap[0:NVH, :, :], in_=ps[0:NVH, bk, 0:256].rearrange("p (a c) -> p a c", a=2), func=AF.Copy)
                for kl in range(4):
                    qT_, kT_ = pk[kl][par]["qT"], pk[kl][par]["kT"]
                    c_ = pkc[kl]
                    bk = self.nb()
                    self.mm(ps[:, bk, 0:128], [(kT_.ap, kT_.ap)], [kT_.key], psk(bk))
                    self.mm(ps[:, bk, 128:256], [(kT_.ap, qT_.ap)], [kT_.key, qT_.key], psk(bk))
                    self.tr(ps[:, bk, 256:384], kT_.ap, I_[:], [kT_.key, "ident_f"], psk(bk))
                    self.v("dve", "tensor_tensor", psk(bk) + ["g_mus"], [c_["kkm"].key], out=c_["kkm"].ap, in0=ps[:, bk, 0:128], in1=self.g_mus[:], op=ALU.mult)
                    self.v("dve", "tensor_tensor", psk(bk) + ["g_mu"], [c_["qkm"].key], out=c_["qkm"].ap, in0=ps[:, bk, 128:256], in1=self.g_mu[:], op=ALU.mult)
                    self.v("act", "activation", psk(bk), [c_["ktm"].key], out=c_["ktm"].ap, in_=ps[:, bk, 256:384], func=AF.Copy)
                for hl in range(8):
                    h = grp * 8 + hl
                    kl = hl // 2
                    st = sets[hl % NS]
                    p_ = ph[hl][par]
                    c_ = pkc[kl]
                    qT_ = pk[kl][par]["qT"]
                    gk = [("g_beta", i), ("g_gcum", i), ("g_bexp", i), ("g_kdsc", i)]
                    bk = self.nb()
                    self.tr(ps[:, bk, 0:128], vT[hl][par].ap, I_[:], [vT[hl][par].key, "ident_f"], psk(bk))
                    self.v("act", "activation", psk(bk) + gk, [st["vb"].key], out=st["vb"].ap, in_=ps[:, bk, 0:128], func=AF.Copy, scale=self.g_beta[:, i, h:h + 1])
                    self.v("dve", "tensor_scalar", [c_["ktm"].key] + gk, [st["kbg"].key], out=st["kbg"].ap, in0=c_["ktm"].ap, scalar1=self.g_bexp[:, i, h:h + 1], scalar2=None, op0=ALU.mult)
                    self.v("dve", "tensor_scalar", [c_["ktm"].key] + gk, [p_["kdec"].key], out=p_["kdec"].ap, in0=c_["ktm"].ap, scalar1=self.g_kdsc[:, i, h:h + 1], scalar2=None, op0=ALU.mult)
                    bk = self.nb()
                    self.mm(ps[:, bk, 0:128], [(self.g_sel[:, h, :], bT[par].ap[0:NVH, 1, :])], ["g_sel", bT[par].key], psk(bk))
                    self.mm(ps[:, bk, 128:256], [(self.g_sel[:, h, :], bT[par].ap[0:NVH, 0, :])], ["g_sel", bT[par].key], psk(bk))
                    self.v("dve", "tensor_scalar", psk(bk) + gk, [st["e1"].key], out=st["e1"].ap, in0=ps[:, bk, 0:128], scalar1=self.g_gcum[:, i, h:h + 1], scalar2=0.0,
                           op0=ALU.subtract, op1=ALU.min)
                    self.v("act", "activation", [st["e1"].key], [st["dec"].key], out=st["dec"].ap, in_=st["e1"].ap, func=AF.Exp)
                    self.v("act", "activation", psk(bk), [p_["eg"].key], out=p_["eg"].ap, in_=ps[:, bk, 0:128], func=AF.Exp)
                    self.v("pool", "tensor_tensor", [c_["qkm"].key, st["dec"].key], [p_["attnT"].key], out=p_["attnT"].ap, in0=c_["qkm"].ap, in1=st["dec"].ap, op=ALU.mult)
                    self.v("pool", "tensor_tensor", [c_["kkm"].key, st["dec"].key], [st["at"].key], out=st["at"].ap, in0=c_["kkm"].ap, in1=st["dec"].ap, op=ALU.mult)
                    self.v("dve", "tensor_tensor", psk(bk) + [st["at"].key], [st["AT"].key], out=st["AT"].ap, in0=st["at"].ap, in1=ps[:, bk, 128:256], op=ALU.mult)
                    self.v("dve", "tensor_tensor", [qT_.key, p_["eg"].key], [p_["qdT"].key], out=p_["qdT"].ap, in0=qT_.ap, in1=p_["eg"].ap, op=ALU.mult)
                    bk = self.nb()
                    self.tr(ps[:, bk, 0:128], st["AT"].ap, I_[:], [st["AT"].key, "ident_f"], psk(bk))
                    self.v("act", "activation", psk(bk), [st["Am"].key], out=st["Am"].ap, in_=ps[:, bk, 0:128], func=AF.Copy)
                    self.v("pool", "tensor_tensor", [st["AT"].key, "ident_f"], [st["R"].key], out=st["R"].ap, in0=I_[:], in1=st["AT"].ap, op=ALU.subtract)
                    X, XT_ = st["Am"], st["AT"]
                    for k in range(1, 6):
                        Pn, PTn = st["P%d" % (k % 2)], st["PT%d" % (k % 2)]
                        bk = self.nb()
                        self.mm(ps[:, bk, 0:128], [(XT_.ap, X.ap)], [X.key, XT_.key], psk(bk))
                        if k < 5:
                            self.mm(ps[:, bk, 128:256], [(X.ap, XT_.ap)], [X.key, XT_.key], psk(bk))
                        self.v("act", "activation", psk(bk), [Pn.key], out=Pn.ap, in_=ps[:, bk, 0:128], func=AF.Copy)
                        if k < 5:
                            self.v("dve", "tensor_copy", psk(bk), [PTn.key], out=PTn.ap, in_=ps[:, bk, 128:256])
                        b2 = self.nb()
                        self.mm(ps[:, b2, 0:128], [(Pn.ap, st["R"].ap)], [Pn.key, st["R"].key], psk(b2))
                        self.v("dve", "tensor_tensor", psk(b2) + [st["R"].key], [st["R"].key], out=st["R"].ap, in0=st["R"].ap, in1=ps[:, b2, 0:128], op=ALU.add)
                        X, XT_ = Pn, PTn
                    bk = self.nb()
                    self.mm(ps[:, bk, 0:128], [(st["R"].ap, st["vb"].ap)], [st["R"].key, st["vb"].key], psk(bk))
                    self.mm(ps[:, bk, 128:256], [(st["kbg"].ap, st["R"].ap)], [st["R"].key, st["kbg"].key], psk(bk))
                    self.v("act", "activation", psk(bk), [p_["u"].key], out=p_["u"].ap, in_=ps[:, bk, 0:128], func=AF.Copy)
                    self.v("dve", "tensor_copy", psk(bk), [p_["wT"].key], out=p_["wT"].ap, in_=ps[:, bk, 128:256])
                for ck in range(2):
                    r0, r1 = ck * 64, (ck + 1) * 64
                    Mr = r1
                    for hl in range(8):
                        p_ = ph[hl][par]
                        Sh = S.ap[:, hl, :]
                        sk = (S.key, hl)
                        bk = self.nb()
                        self.mm(ps[0:Mr, bk, 0:128], [(p_["wT"].ap[:, 0:Mr], Sh)], [p_["wT"].key, sk], psk(bk))
                        self.v("dve", "tensor_tensor", psk(bk) + [p_["u"].key], [(vn[hl].key, ck)], out=vn[hl].ap[r0:r1, :], in0=p_["u"].ap[r0:r1, :], in1=ps[r0:r1, bk, 0:128], op=ALU.subtract)
                        bk = self.nb()
                        self.mm(ps[0:Mr, bk, 0:128], [(p_["qdT"].ap[:, 0:Mr], Sh), (p_["attnT"].ap[r0:r1, 0:Mr], vn[hl].ap[r0:r1, :])],
                                [p_["qdT"].key, sk, p_["attnT"].key, (vn[hl].key, ck)], psk(bk))
                        self.v("act", "activation", psk(bk), [(o_tm.key, hl, ck)], out=o_tm.ap[r0:r1, hl, :], in_=ps[r0:r1, bk, 0:128], func=AF.Copy)
                        bk = self.nb()
                        self.mm(ps[:, bk, 0:128], [(p_["kdec"].ap[r0:r1, :], vn[hl].ap[r0:r1, :])], [p_["kdec"].key, (vn[hl].key, ck)], psk(bk))
                        self.v("dve", "scalar_tensor_tensor", psk(bk) + [sk, p_["eg"].key], [sk], out=Sh, in0=Sh, scalar=p_["eg"].ap[:, r1 - 1:r1], in1=ps[:, bk, 0:128],
                               op0=ALU.mult, op1=ALU.add)
                okeys = [(o_tm.key, hl, ck) for hl in range(8) for ck in range(2)]
                self.v("pool", "tensor_tensor", okeys, [sqo.key], out=sqo.ap, in0=o_tm.ap, in1=o_tm.ap, op=ALU.mult)
                self.v("dve", "tensor_reduce", [sqo.key], [ssum.key], out=ssum.ap, in_=sqo.ap, axis=AX.X, op=ALU.add)
                self.v("act", "activation", [ssum.key], [ssum.key], out=ssum.ap, in_=ssum.ap, func=AF.Sqrt, scale=1.0 / 128.0, bias=1.0e-6)
                self.v("dve", "reciprocal", [ssum.key], [ssum.key], out=ssum.ap, in_=ssum.ap)
                self.v("dve", "tensor_tensor", okeys + [ssum.key], [sqo.key], out=sqo.ap, in0=o_tm.ap, in1=ssum.ap.unsqueeze(2).to_broadcast([128, 8, 128]), op=ALU.mult)
                self.v("pool", "tensor_tensor", [sqo.key, ng.key], [sqo.key], out=sqo.ap, in0=sqo.ap, in1=ng.ap.unsqueeze(1).to_broadcast([128, 8, 128]), op=ALU.mult)
                self.v("dve", "tensor_tensor", [sqo.key, zs[par].key], [ogb.key], out=ogb.ap, in0=sqo.ap, in1=zs[par].ap, op=ALU.mult)
                bk = self.nb()
                for hl in range(8):
                    self.tr(psb[:, bk, hl * 128:(hl + 1) * 128], ogb.ap[:, hl, :], self.ident_b[:], [ogb.key, "ident_b"], psk(bk))
                self.v("act", "activation", psk(bk), [ogT[par].key], out=ogT[par].ap, in_=psb[:, bk, :].rearrange("p (a c) -> p a c", a=8), func=AF.Copy)
                self.dma(oT_v[:, grp * 8:(grp + 1) * 8, tsl], ogT[par].ap, [ogT[par].key], [("oT_g", grp, i)])

    def gdn_outproj_hfn(self, j, W, oT_g, ntiles=NT):
        ps = self.ps
        wo = self.abuf("g_wo", [16, D], BF16)
        self.cast_load(wo.ap, W["gdn_w_out"][j].rearrange("(kc p) f -> p kc f", p=128), [], [wo.key])
        ot = [self.abuf("g_ot%d" % s, [16, 128], BF16) for s in range(2)]
        oT_v = oT_g.rearrange("(h p) t -> p h t", p=128)

        def h_fn(t):
            o_ = ot[t % 2]
            self.dma(o_.ap, oT_v[:, :, t * 128:(t + 1) * 128], [("oT_g", 0, t), ("oT_g", 1, t)], [o_.key])
            res = []
            for dh in range(2):
                bk = self.nb()
                pairs = [(o_.ap[:, kc, :], wo.ap[:, kc, dh * 512:(dh + 1) * 512]) for kc in range(16)]
                self.mm(ps[:, bk, :], pairs, [o_.key, wo.key], psk(bk))
                res.append((ps[:, bk, :], psk(bk)))
            return res
        return h_fn


class FullBuilder3(Builder, MoEMixin, SBMixin, GDNMixin):
    sub = 'z'


WSHAPES = {
    "gdn_w_in": [2, 1024, 6176], "gdn_conv": [2, 4, 4096], "gdn_a_log": [2, 16], "gdn_dt_bias": [2, 16], "gdn_norm_g": [2, 128],
    "gdn_w_out": [2, 2048, 1024], "sb_w_qkv": [2, 1024, 3072], "sb_w_out": [2, 1024, 1024], "ln1_g": [4, 1024], "ln1_b": [4, 1024],
    "moe_w_router": [4, 1024, 32], "moe_b_router": [4, 32], "moe_w_gu": [4, 32, 1024, 2048], "moe_b_gu": [4, 32, 2048],
    "moe_w_down": [4, 32, 1024, 1024], "moe_b_down": [4, 32, 1024], "ln2_g": [4, 1024], "ln2_b": [4, 1024],
}


def build_full(depth=4):
    nc = bass.Bass("TRN2", target_bir_lowering=False)
    W = {k: nc.dram_tensor(k, sh, F32, kind="ExternalInput").ap() for k, sh in WSHAPES.items()}
    x = nc.dram_tensor("x", [T, D], F32, kind="ExternalInput").ap()
    out = nc.dram_tensor("out", [T, D], F32, kind="ExternalOutput").ap()
    I = lambda name, sh, dt: nc.dram_tensor(name, sh, dt, kind="Internal").ap()
    xres = I("xres", [T, D], F32)
    xrows = I("xrows", [NROWS, D], BF16)
    yrows = I("yrows", [NROWS, D], F32)
    qkvT_g = I("qkvT_g", [4096, T], F32)
    zs_g = I("zs_g", [T, 2048], F32)
    oT_g = I("oT_g", [2048, T], BF16)
    qT_d = I("qT_d", [D, T], BF16)
    kT_d = I("kT_d", [D, T], BF16)
    v_d = I("v_d", [T, D], BF16)
    with ExitStack() as es:
        B = FullBuilder3(nc, es, arena_kib=140)
        full_words = B.A.words
        B.alloc_moe_state()
        B.gdn_consts()
        B.sb_consts()
        B.zero_rows(xrows)
        B.A.reset(); B.P.barrier()
        XT = xt_view(B)
        B.A.words = full_words - XT_WORDS
        xt_from_x(B, x, XT)
        for l in range(depth):
            j = l // 2
            B.load_layer_small(l, W)
            B.A.words = full_words - XT_WORDS
            B.A.reset(); B.P.barrier()
            if l % 2 == 0:
                B.gdn_g1(j, W, XT, qkvT_g, zs_g)
                B.A.words = full_words
                B.A.reset(); B.P.barrier()
                B.gdn_g2(j, W, qkvT_g, zs_g, oT_g)
                B.A.reset(); B.P.barrier()
                h_fn = B.gdn_outproj_hfn(j, W, oT_g)
            else:
                B.sb_proj(j, W, XT, qT_d, kT_d, v_d)
                B.A.reset(); B.P.barrier()
                OT = XT
                B.sb_attn(qT_d, kT_d, v_d, OT)
                B.A.reset(); B.P.barrier()
                h_fn = B.sb_outproj_hfn(j, W, OT, [])
            B.post_mixer_phase(l, x if l == 0 else xres, xres, h_fn=h_fn)
            B.A.words = full_words
            last = (l == depth - 1)
            B.moe_layer(l, W, xres, xrows, yrows, out if last else xres, XT=None if last else XT, xt_words=XT_WORDS)
        stats = B.P.emit(es)
    return nc, stats


_NC_CACHE = {}


def kernel(**inputs):
    if "nc" not in _NC_CACHE:
        _NC_CACHE["nc"] = build_full()[0]
    nc = _NC_CACHE["nc"]
    x = np.ascontiguousarray(np.asarray(inputs["x"], dtype=np.float32))
    nb = x.shape[0]
    ws = {k: np.ascontiguousarray(np.asarray(inputs[k], dtype=np.float32)) for k in WSHAPES}
    in_maps = []
    for b in range(nb):
        m = {"x": x[b]}
        m.update(ws)
        in_maps.append(m)
    res = run_bass_kernel_spmd(nc, in_maps, core_ids=list(range(nb)))
    return np.stack([np.asarray(r["out"]) for r in res.results], axis=0).astype(np.float32)
```
